# Optimizing a Trainium2 kernel written in Bass

```python
import jax, jax.numpy as jnp
from jax import lax
import numpy as np

D_MODEL = 1024
BATCH = 2
SEQ = 8192
DEPTH = 2

GRID_W = 64
CTX_LEN = 256
CONV_DIM = 256
CONV_GROUPS = 4
CONV_K = 3
POOL_DIM = 256
POOL_WINDOWS = (2, 4, 8, 16)
N_POOL = 4
POOL_GROUP_DIM = POOL_DIM // N_POOL
HEAD_DIM = 64
GQA_HEADS = 4
GQA_KV_HEADS = 2
GQA_DIM = GQA_HEADS * HEAD_DIM
GQA_SCALE = HEAD_DIM ** -0.5
MLA_HEADS = 4
MLA_NOPE_DIM = 64
MLA_ROPE_DIM = 32
MLA_QK_DIM = MLA_NOPE_DIM + MLA_ROPE_DIM
MLA_V_DIM = 64
MLA_Q_RANK = 256
MLA_KV_RANK = 128
MLA_DIM = MLA_HEADS * MLA_V_DIM
MLA_SCALE = MLA_QK_DIM ** -0.5
IN_LAYOUT = (("conv", 3 * CONV_DIM), ("pool", POOL_DIM), ("gqa_q", GQA_HEADS * HEAD_DIM),
             ("gqa_kv", 2 * GQA_KV_HEADS * HEAD_DIM), ("mla_q", MLA_Q_RANK), ("mla_kv", MLA_KV_RANK),
             ("mla_kr", MLA_ROPE_DIM))
D_IN = 3 * CONV_DIM + POOL_DIM + GQA_HEADS * HEAD_DIM + 2 * GQA_KV_HEADS * HEAD_DIM + MLA_Q_RANK + MLA_KV_RANK + MLA_ROPE_DIM
D_MIX = CONV_DIM + POOL_DIM + GQA_DIM + MLA_DIM
N_EXPERTS = 16
N_EXPERT_GROUPS = 4
EXPERTS_PER_GROUP = N_EXPERTS // N_EXPERT_GROUPS
TOP_K = 2
D_EXPERT = 1024
MOE_BLOCK = 256
ROPE_THETA = 10000.0
NORM_EPS = 1e-6
Q_BLOCK = 128

kernel_name = "hybrid_parallel_heads_dit_moe"


def rms_norm(t, gain):
    tf = t.astype(jnp.float32)
    y = tf * lax.rsqrt(jnp.mean(tf * tf, axis=-1, keepdims=True) + NORM_EPS)
    return (y * gain.astype(jnp.float32)).astype(t.dtype)


def modulate(t, shift, scale):
    return t * (1.0 + scale) + shift


def adaln(cvec, w_mod, b_mod, n_chunks):
    n = n_chunks * w_mod.shape[0]
    m = jax.nn.silu(cvec) @ w_mod[:, :n] + b_mod[:n]
    return jnp.split(m, n_chunks, axis=-1)


def in_offsets():
    out, off = {}, 0
    for name, n in IN_LAYOUT:
        out[name] = (off, n)
        off += n
    return out


def split_in(z):
    return {name: z[..., off:off + n] for name, (off, n) in in_offsets().items()}


def col_block(w, name):
    off, n = in_offsets()[name]
    return w[:, off:off + n]


def axial_rope(row_id, col_id, rot_dim):
    n_freq = rot_dim // 4
    inv_freq = jnp.power(ROPE_THETA, -jnp.arange(n_freq, dtype=jnp.float32) / n_freq)
    ang = jnp.concatenate([row_id[:, None] * inv_freq, col_id[:, None] * inv_freq], axis=-1)
    return jnp.cos(ang), jnp.sin(ang)


def apply_rope(t, cos, sin):
    half = t.shape[-1] // 2
    t1, t2 = t[..., :half], t[..., half:]
    cs = cos[None, :, None, :].astype(t.dtype)
    sn = sin[None, :, None, :].astype(t.dtype)
    return jnp.concatenate([t1 * cs - t2 * sn, t1 * sn + t2 * cs], axis=-1)


def rope_tail(t, cos, sin):
    return jnp.concatenate([t[..., :MLA_NOPE_DIM], apply_rope(t[..., MLA_NOPE_DIM:], cos, sin)], axis=-1)


def attend(q, k, v, scale):
    b, nq, hk, g, dq = q.shape
    nb = nq // Q_BLOCK
    qb = jnp.moveaxis(q.reshape(b, nb, Q_BLOCK, hk, g, dq), 1, 0)

    def one_block(qblk):
        s = jnp.einsum("bqhgd,bkhd->bhgqk", qblk, k).astype(jnp.float32) * scale
        p = jax.nn.softmax(s, axis=-1).astype(v.dtype)
        return jnp.einsum("bhgqk,bkhd->bqhgd", p, v)

    o = lax.map(one_block, qb)
    return jnp.moveaxis(o, 0, 1).reshape(b, nq, hk, g, v.shape[-1])


def short_conv_mixer(z, conv_w):
    b_gate, c_gate, u = jnp.split(z, 3, axis=-1)
    vp = jnp.pad(c_gate * u, ((0, 0), (1, 1), (0, 0)))
    conv = vp[:, :-2] * conv_w[0] + vp[:, 1:-1] * conv_w[1] + vp[:, 2:] * conv_w[2]
    return b_gate * conv


def pool_mixer(z, w_pool, pool_scale):
    b, n, _ = z.shape
    zf = z.astype(jnp.float32)
    cs = jnp.concatenate([jnp.zeros((b, 1, POOL_DIM), jnp.float32), jnp.cumsum(zf, axis=1)], axis=1)
    t = jnp.arange(n)
    outs = []
    for g, w in enumerate(POOL_WINDOWS):
        lo_c, hi_c = g * POOL_GROUP_DIM, (g + 1) * POOL_GROUP_DIM
        lo = jnp.clip(t - w // 2, 0, n)
        hi = jnp.clip(t + w // 2, 0, n)
        csg = cs[..., lo_c:hi_c]
        mean = (csg[:, hi] - csg[:, lo]) / (hi - lo).astype(jnp.float32)[None, :, None]
        d = (mean - zf[..., lo_c:hi_c]).astype(z.dtype)
        outs.append(d @ w_pool[g])
    return jnp.concatenate(outs, axis=-1) * pool_scale


def gqa_queries(zq, gain):
    b, n, _ = zq.shape
    return rms_norm(zq.reshape(b, n, GQA_HEADS, HEAD_DIM), gain)


def gqa_keys_values(zkv, gain):
    b, n, _ = zkv.shape
    k, v = jnp.split(zkv, 2, axis=-1)
    k = rms_norm(k.reshape(b, n, GQA_KV_HEADS, HEAD_DIM), gain)
    return k, v.reshape(b, n, GQA_KV_HEADS, HEAD_DIM)


def group_heads(q):
    b, n, h, d = q.shape
    return q.reshape(b, n, GQA_KV_HEADS, h // GQA_KV_HEADS, d)


def mla_queries(zq, q_norm, w_uq, qk_gain):
    b, n, _ = zq.shape
    q = (rms_norm(zq, q_norm) @ w_uq).reshape(b, n, MLA_HEADS, MLA_QK_DIM)
    return rms_norm(q, qk_gain)


def mla_keys_values(zkv, zkr, kv_norm, w_uk, w_uv, qk_gain):
    b, n, _ = zkv.shape
    ckv = rms_norm(zkv, kv_norm)
    k_nope = (ckv @ w_uk).reshape(b, n, MLA_HEADS, MLA_NOPE_DIM)
    v = (ckv @ w_uv).reshape(b, n, MLA_HEADS, MLA_V_DIM)
    k_rope = jnp.broadcast_to(zkr[:, :, None, :], (b, n, MLA_HEADS, MLA_ROPE_DIM))
    k = rms_norm(jnp.concatenate([k_nope, k_rope], axis=-1), qk_gain)
    return k, v


def moe_ffn(h, w_router, b_router, w_gate, w_up, w_down):
    t_tok, d = h.shape
    scores = jax.nn.sigmoid(h.astype(jnp.float32) @ w_router.astype(jnp.float32))
    sel = (scores + b_router.astype(jnp.float32)).reshape(t_tok, N_EXPERT_GROUPS, EXPERTS_PER_GROUP)
    group_score = lax.top_k(sel, 2)[0].sum(-1)
    g_best = jnp.argmax(group_score, axis=-1)
    in_group = jnp.take_along_axis(sel, g_best[:, None, None], axis=1)[:, 0]
    _, local = lax.top_k(in_group, TOP_K)
    idx = g_best[:, None] * EXPERTS_PER_GROUP + local
    gw = jnp.take_along_axis(scores, idx, axis=-1)
    gw = gw / jnp.sum(gw, axis=-1, keepdims=True)
    tk = t_tok * TOP_K
    flat_e = idx.reshape(tk)
    flat_t = jnp.arange(tk) // TOP_K
    flat_w = gw.reshape(tk)
    order = jnp.argsort(flat_e)
    se, st, sw = flat_e[order], flat_t[order], flat_w[order]
    counts = jnp.bincount(flat_e, length=N_EXPERTS)
    padded = ((counts + MOE_BLOCK - 1) // MOE_BLOCK) * MOE_BLOCK
    pad_end = jnp.cumsum(padded)
    pad_start = pad_end - padded
    start = jnp.cumsum(counts) - counts
    dest = pad_start[se] + jnp.arange(tk) - start[se]
    n_blocks = -(-tk // MOE_BLOCK) + N_EXPERTS
    slot_tok = jnp.zeros((n_blocks * MOE_BLOCK,), jnp.int32).at[dest].set(st.astype(jnp.int32))
    block_exp = jnp.minimum(jnp.searchsorted(pad_end, jnp.arange(n_blocks) * MOE_BLOCK, side="right"), N_EXPERTS - 1)
    xb = h[slot_tok].reshape(n_blocks, MOE_BLOCK, d)

    def expert_block(args):
        xblk, e = args
        a = xblk @ w_gate[e]
        u = xblk @ w_up[e]
        return (jax.nn.silu(a) * u) @ w_down[e]

    yb = lax.map(expert_block, (xb, block_exp)).reshape(n_blocks * MOE_BLOCK, d)
    y_assign = yb[dest] * sw[:, None].astype(h.dtype)
    return jax.ops.segment_sum(y_assign, st, num_segments=t_tok)


def setup_inputs(seed: int = 0) -> dict:
    key = jax.random.key(seed)
    ks = jax.random.split(key, 32)
    f32 = jnp.float32
    D = D_MODEL

    def nrm(k, shape, scale):
        return jax.random.normal(k, shape, f32) * scale

    def gain(k, shape, s=0.02):
        return 1.0 + s * jax.random.normal(k, shape, f32)

    return {
        "x": nrm(ks[0], (BATCH, SEQ, D), 1.0),
        "c": nrm(ks[1], (BATCH, D), 1.0),
        "ctx": nrm(ks[2], (BATCH, CTX_LEN, D), 1.0),
        "c_ctx": nrm(ks[3], (D,), 1.0),
        "w_mod": nrm(ks[4], (DEPTH, D, 6 * D), 0.5 * D ** -0.5),
        "b_mod": nrm(ks[5], (DEPTH, 6 * D), 0.02),
        "norm1": gain(ks[6], (DEPTH, D)),
        "norm2": gain(ks[7], (DEPTH, D)),
        "w_in": nrm(ks[8], (DEPTH, D, D_IN), D ** -0.5),
        "conv_w": nrm(ks[9], (DEPTH, CONV_K, CONV_DIM), CONV_K ** -0.5),
        "w_pool": nrm(ks[10], (DEPTH, N_POOL, POOL_GROUP_DIM, POOL_GROUP_DIM), POOL_GROUP_DIM ** -0.5),
        "pool_scale": gain(ks[11], (DEPTH, POOL_DIM), 0.1),
        "gqa_q_norm": gain(ks[12], (DEPTH, HEAD_DIM)),
        "gqa_k_norm": gain(ks[13], (DEPTH, HEAD_DIM)),
        "mla_q_norm": gain(ks[14], (DEPTH, MLA_Q_RANK)),
        "mla_kv_norm": gain(ks[15], (DEPTH, MLA_KV_RANK)),
        "mla_w_uq": nrm(ks[16], (DEPTH, MLA_Q_RANK, MLA_HEADS * MLA_QK_DIM), MLA_Q_RANK ** -0.5),
        "mla_w_uk": nrm(ks[17], (DEPTH, MLA_KV_RANK, MLA_HEADS * MLA_NOPE_DIM), MLA_KV_RANK ** -0.5),
        "mla_w_uv": nrm(ks[18], (DEPTH, MLA_KV_RANK, MLA_HEADS * MLA_V_DIM), MLA_KV_RANK ** -0.5),
        "mla_qk_q_norm": gain(ks[19], (DEPTH, MLA_QK_DIM)),
        "mla_qk_k_norm": gain(ks[20], (DEPTH, MLA_QK_DIM)),
        "w_out": nrm(ks[21], (DEPTH, D_MIX, D), D_MIX ** -0.5),
        "w_router": nrm(ks[22], (D, N_EXPERTS), D ** -0.5),
        "b_router": nrm(ks[23], (N_EXPERTS,), 0.01),
        "w_gate": nrm(ks[24], (DEPTH, N_EXPERTS, D, D_EXPERT), D ** -0.5),
        "w_up": nrm(ks[25], (DEPTH, N_EXPERTS, D, D_EXPERT), D ** -0.5),
        "w_down": nrm(ks[26], (DEPTH, N_EXPERTS, D_EXPERT, D), D_EXPERT ** -0.5),
    }


def reference(x, c, ctx, c_ctx, w_mod, b_mod, norm1, norm2, w_in, conv_w, w_pool, pool_scale,
              gqa_q_norm, gqa_k_norm, mla_q_norm, mla_kv_norm, mla_w_uq, mla_w_uk, mla_w_uv,
              mla_qk_q_norm, mla_qk_k_norm, w_out, w_router, b_router, w_gate, w_up, w_down):
    b, n_lat, d = x.shape
    n_ctx = ctx.shape[1]
    rows = n_lat // GRID_W
    row_id = jnp.repeat(jnp.arange(rows, dtype=jnp.float32), GRID_W)
    col_id = jnp.tile(jnp.arange(GRID_W, dtype=jnp.float32), rows)
    g_cos, g_sin = axial_rope(row_id, col_id, HEAD_DIM)
    m_cos, m_sin = axial_rope(row_id, col_id, MLA_ROPE_DIM)
    xc = ctx
    for i in range(DEPTH):
        last = i == DEPTH - 1
        sh1, sc1, g1, sh2, sc2, g2 = adaln(c, w_mod[i], b_mod[i], 6)
        cm = adaln(c_ctx, w_mod[i], b_mod[i], 2 if last else 6)
        h = modulate(rms_norm(x, norm1[i]), sh1[:, None], sc1[:, None])
        hc = modulate(rms_norm(xc, norm1[i]), cm[0], cm[1])
        zl = split_in(h @ w_in[i])
        if last:
            zc = {nm: hc @ col_block(w_in[i], nm) for nm in ("gqa_kv", "mla_kv", "mla_kr")}
        else:
            zc = split_in(hc @ w_in[i])
        kc_g, vc_g = gqa_keys_values(zc["gqa_kv"], gqa_k_norm[i])
        kc_m, vc_m = mla_keys_values(zc["mla_kv"], zc["mla_kr"], mla_kv_norm[i], mla_w_uk[i], mla_w_uv[i], mla_qk_k_norm[i])
        y_conv = short_conv_mixer(zl["conv"], conv_w[i])
        y_pool = pool_mixer(zl["pool"], w_pool[i], pool_scale[i])
        q_g = apply_rope(gqa_queries(zl["gqa_q"], gqa_q_norm[i]), g_cos, g_sin)
        k_g, v_g = gqa_keys_values(zl["gqa_kv"], gqa_k_norm[i])
        k_g = apply_rope(k_g, g_cos, g_sin)
        y_gqa = attend(group_heads(q_g), jnp.concatenate([kc_g, k_g], axis=1),
                       jnp.concatenate([vc_g, v_g], axis=1), GQA_SCALE).reshape(b, n_lat, GQA_DIM)
        q_m = rope_tail(mla_queries(zl["mla_q"], mla_q_norm[i], mla_w_uq[i], mla_qk_q_norm[i]), m_cos, m_sin)
        k_m, v_m = mla_keys_values(zl["mla_kv"], zl["mla_kr"], mla_kv_norm[i], mla_w_uk[i], mla_w_uv[i], mla_qk_k_norm[i])
        k_m = rope_tail(k_m, m_cos, m_sin)
        y_mla = attend(q_m[:, :, :, None], jnp.concatenate([kc_m, k_m], axis=1),
                       jnp.concatenate([vc_m, v_m], axis=1), MLA_SCALE).reshape(b, n_lat, MLA_DIM)
        y = jnp.concatenate([y_conv, y_pool, y_gqa, y_mla], axis=-1) @ w_out[i]
        x = x + g1[:, None] * y
        if not last:
            yc_conv = short_conv_mixer(zc["conv"], conv_w[i])
            yc_pool = pool_mixer(zc["pool"], w_pool[i], pool_scale[i])
            qc_g = gqa_queries(zc["gqa_q"], gqa_q_norm[i])
            yc_gqa = attend(group_heads(qc_g), kc_g, vc_g, GQA_SCALE).reshape(b, n_ctx, GQA_DIM)
            qc_m = mla_queries(zc["mla_q"], mla_q_norm[i], mla_w_uq[i], mla_qk_q_norm[i])
            yc_mla = attend(qc_m[:, :, :, None], kc_m, vc_m, MLA_SCALE).reshape(b, n_ctx, MLA_DIM)
            yc = jnp.concatenate([yc_conv, yc_pool, yc_gqa, yc_mla], axis=-1) @ w_out[i]
            xc = xc + cm[2] * yc
        h2 = modulate(rms_norm(x, norm2[i]), sh2[:, None], sc2[:, None])
        tok = h2.reshape(b * n_lat, d)
        if not last:
            h2c = modulate(rms_norm(xc, norm2[i]), cm[3], cm[4])
            tok = jnp.concatenate([tok, h2c.reshape(b * n_ctx, d)], axis=0)
        y2 = moe_ffn(tok, w_router, b_router, w_gate[i], w_up[i], w_down[i])
        x = x + g2[:, None] * y2[:b * n_lat].reshape(b, n_lat, d)
        if not last:
            xc = xc + cm[5] * y2[b * n_lat:].reshape(b, n_ctx, d)
    return x
```

```python
import numpy as np
import ml_dtypes
from contextlib import ExitStack
import concourse.bass as bass
import concourse.mybir as mybir
from concourse.bass_utils import run_bass_kernel_spmd

F32 = mybir.dt.float32
BF16 = mybir.dt.bfloat16
I32 = mybir.dt.int32
ALU = mybir.AluOpType
AF = mybir.ActivationFunctionType
AX = mybir.AxisListType


def _dsz(dt):
    s = str(dt)
    if "bfloat16" in s or "float16" in s or "int16" in s:
        return 2
    if "8" in s and "float8" in s or s.endswith("int8"):
        return 1
    return 4


class _Op:
    __slots__ = ("eng", "fn", "deps", "key", "isdma", "val", "sem", "need")


class Sched:
    ENGS = ("pe", "act", "dve", "pool", "sp")
    NSLOT = 6

    def __init__(self, nc):
        self.nc = nc
        self.ops = []
        self.recs = {}
        self.mcache = {}
        self.rr = {"sp": 0, "pool": 0, "act": 0}
        self.last_dma = {}

    def _region(self, ap):
        t = ap.tensor
        tn = type(t).__name__
        dsz = _dsz(ap.dtype)
        pairs = [(int(s), int(c)) for s, c in ap.ap]
        off = int(ap.offset)
        if tn.startswith("DRam"):
            ext = sum((c - 1) * abs(s) for s, c in pairs) + 1
            return ("D", t.name), (0, 1, off * dsz, (off + ext) * dsz)
        key = t.name
        m = self.mcache.get(key)
        if m is None:
            ml = self.nc.lookup_mloc(t)
            rowelems = 1
            for d in t.shape[1:]:
                rowelems *= int(d)
            tdsz = _dsz(t.dtype)
            base = int(ml.addr) + (int(ml.bank) * 2048 if tn.startswith("PSum") else 0)
            m = (base, rowelems, tdsz)
            self.mcache[key] = m
        base, rowelems, tdsz = m
        rowe = rowelems * tdsz // dsz
        p0 = off // rowe
        f0 = off % rowe
        ps, pc = pairs[0]
        ext = sum((c - 1) * abs(s) for s, c in pairs[1:]) + 1
        if tn.startswith("PSum"):
            b0 = base + f0 * dsz
            b1 = base + (f0 + ext) * dsz
            return "P", (0, 128, (b0 // 2048) * 2048, ((b1 + 2047) // 2048) * 2048)
        return "S", (p0, p0 + pc, base + f0 * dsz, base + (f0 + ext) * dsz)

    def _add(self, eng, fn, reads, writes, isdma=False, slot=None):
        op = _Op()
        op.eng = eng
        op.fn = fn
        op.isdma = isdma
        op.key = ("dma", eng, slot) if isdma else eng
        op.val = None
        op.sem = None
        op.need = False
        idx = len(self.ops)
        deps = {}

        def dep(j):
            if j is None:
                return
            k = self.ops[j].key
            if k == "pe" and op.key == "pe":
                return
            if deps.get(k, -1) < j:
                deps[k] = j

        rregs = [self._region(a) for a in reads]
        wregs = [self._region(a) for a in writes]
        wregs = wregs + [r for r in rregs if r[0] == "P"]
        rregs = [r for r in rregs if r[0] != "P"]
        for sp, r in rregs:
            for rec in self.recs.get(sp, ()):
                if rec[0] < r[1] and r[0] < rec[1] and rec[2] < r[3] and r[2] < rec[3]:
                    dep(rec[4])
        for sp, r in wregs:
            for rec in self.recs.get(sp, ()):
                if rec[0] < r[1] and r[0] < rec[1] and rec[2] < r[3] and r[2] < rec[3]:
                    dep(rec[4])
                    for j in rec[5].values():
                        dep(j)
        if isdma:
            dep(self.last_dma.get(op.key))
            self.last_dma[op.key] = idx
        for sp, r in rregs:
            hit = False
            for rec in self.recs.get(sp, ()):
                if rec[0] < r[1] and r[0] < rec[1] and rec[2] < r[3] and r[2] < rec[3]:
                    rec[5][op.key] = idx
                    hit = True
            if not hit and sp != "S" and sp != "P":
                pass
            if not hit and (sp == "S" or sp == "P"):
                self.recs.setdefault(sp, []).append([r[0], r[1], r[2], r[3], None, {op.key: idx}])
        for sp, r in wregs:
            lst = self.recs.setdefault(sp, [])
            keep = []
            for rec in lst:
                if r[0] <= rec[0] and rec[1] <= r[1] and r[2] <= rec[2] and rec[3] <= r[3]:
                    continue
                keep.append(rec)
            keep.append([r[0], r[1], r[2], r[3], idx, {}])
            self.recs[sp] = keep
        op.deps = deps
        self.ops.append(op)
        return idx

    def mm(self, out, lhsT, rhs, start=True, stop=True):
        return self._add("pe", lambda e: e.matmul(out, lhsT, rhs, start=start, stop=stop),
                         [lhsT, rhs], [out])

    def tr(self, out, in_, ident):
        return self._add("pe", lambda e: e.transpose(out, in_, ident), [in_, ident], [out])

    def act(self, out, in_, func, bias=None, scale=None, accum=None):
        kw = {}
        rd = [in_]
        wr = [out]
        if bias is not None:
            kw["bias"] = bias
            if not isinstance(bias, (int, float)):
                rd.append(bias)
        if scale is not None:
            kw["scale"] = scale
            if not isinstance(scale, (int, float)):
                rd.append(scale)
        if accum is not None:
            kw["accum_out"] = accum
            wr.append(accum)
        return self._add("act", lambda e: e.activation(out, in_, func, **kw), rd, wr)

    def _e(self, e):
        return {"v": "dve", "g": "pool", "s": "act"}[e]

    def tt(self, e, out, a, b, op):
        return self._add(self._e(e), lambda g: g.tensor_tensor(out, a, b, op), [a, b], [out])

    def ts(self, e, out, a, s1, s2, op0, op1=None):
        rd = [a] + [s for s in (s1, s2) if s is not None and not isinstance(s, (int, float))]
        if op1 is None:
            return self._add(self._e(e), lambda g: g.tensor_scalar(out, a, s1, None, op0), rd, [out])
        return self._add(self._e(e), lambda g: g.tensor_scalar(out, a, s1, s2, op0, op1), rd, [out])

    def stt(self, e, out, a, scalar, b, op0, op1):
        rd = [a, b] + ([] if isinstance(scalar, (int, float)) else [scalar])
        return self._add(self._e(e), lambda g: g.scalar_tensor_tensor(out, a, scalar, b, op0, op1), rd, [out])

    def cp(self, e, out, a):
        if e == "s":
            return self._add("act", lambda g: g.copy(out, a), [a], [out])
        return self._add(self._e(e), lambda g: g.tensor_copy(out, a), [a], [out])

    def ms(self, e, ap, val):
        return self._add(self._e(e), lambda g: g.memset(ap, val), [], [ap])

    def rcp(self, out, a):
        return self._add("dve", lambda g: g.reciprocal(out, a), [a], [out])

    def red(self, out, a, op, axis=AX.X):
        return self._add("dve", lambda g: g.tensor_reduce(out, a, axis, op), [a], [out])

    def dma(self, q, out, in_):
        slot = self.rr[q]
        self.rr[q] = (slot + 1) % self.NSLOT
        return self._add(q, lambda g: g.dma_start(out=out, in_=in_), [in_], [out], isdma=True, slot=slot)

    def custom(self, eng, fn, reads, writes, isdma=False):
        if isdma:
            slot = self.rr[eng]
            self.rr[eng] = (slot + 1) % self.NSLOT
            return self._add(eng, fn, reads, writes, isdma=True, slot=slot)
        return self._add(eng, fn, reads, writes)

    def emit(self, final_wait_eng="sp"):
        nc = self.nc
        ops = self.ops
        for op in ops:
            for j in op.deps.values():
                ops[j].need = True
        lastkey = {}
        for i, op in enumerate(ops):
            lastkey[op.key] = i
        for i in lastkey.values():
            ops[i].need = True
        from contextlib import ExitStack
        with ExitStack() as es:
            sems = {}
            for k in ("pe", "act", "dve", "pool"):
                sems[k] = es.enter_context(nc.semaphore("s_" + k))
            for q in ("sp", "pool", "act"):
                for s in range(self.NSLOT):
                    sems[("dma", q, s)] = es.enter_context(nc.semaphore("d_%s_%d" % (q, s)))
            cnt = {}
            for op in ops:
                if op.isdma:
                    cnt[op.key] = cnt.get(op.key, 0) + 16
                    op.val = cnt[op.key]
                    op.sem = sems[op.key]
                elif op.need:
                    cnt[op.key] = cnt.get(op.key, 0) + 1
                    op.val = cnt[op.key]
                    op.sem = sems[op.key]
            final = dict(lastkey)
            block = es.enter_context(nc.Block())

            def run(engname):
                def body(e):
                    waited = {}
                    for op in ops:
                        if op.eng != engname:
                            continue
                        for k, j in op.deps.items():
                            d = ops[j]
                            if waited.get(k, 0) >= d.val:
                                continue
                            e.wait_ge(d.sem, d.val)
                            waited[k] = d.val
                        ins = op.fn(e)
                        if op.val is not None:
                            ins.then_inc(op.sem, 16 if op.isdma else 1)
                    if engname == final_wait_eng:
                        for k, j in final.items():
                            d = ops[j]
                            if waited.get(k, 0) >= d.val:
                                continue
                            e.wait_ge(d.sem, d.val)
                return body

            block.tensor(run("pe"))
            block.scalar(run("act"))
            block.vector(run("dve"))
            block.gpsimd(run("pool"))
            block.sync(run("sp"))

D = 1024
T = 2304
TL = 2048
NT = 18
BLKS = [(0, 512), (512, 512), (1024, 512), (1536, 512), (2048, 256)]
EPS = 1e-6
NPBF = ml_dtypes.bfloat16
GV0 = 5 * T
MV0 = GV0 + NT * 130
KVC = MV0 + NT * 260
GQA_SCALE = 64 ** -0.5
MLA_SCALE = 96 ** -0.5


class Prog:
    def __init__(self):
        self.nc = bass.Bass("TRN2", target_bir_lowering=False)
        self.S = Sched(self.nc)
        self.es = ExitStack()
        self.rots = {}

    def din(self, name, shape, dt=F32):
        return self.nc.dram_tensor(name, list(shape), dt, kind="ExternalInput").ap()

    def dout(self, name, shape, dt=F32):
        return self.nc.dram_tensor(name, list(shape), dt, kind="ExternalOutput").ap()

    def sb(self, name, shape, dt=F32, es=None):
        return (es or self.es).enter_context(self.nc.sbuf_tensor("s_" + name, list(shape), dt))

    def ps(self, name, shape=(128, 512), dt=F32):
        return self.es.enter_context(self.nc.psum_tensor("p_" + name, list(shape), dt))

    def rot(self, name, n, shape, dt=F32, es=None):
        self.rots[name] = [[self.sb("%s%d" % (name, i), shape, dt, es) for i in range(n)], 0]

    def rotp(self, name, n):
        self.rots[name] = [[self.ps("%s%d" % (name, i)) for i in range(n)], 0]

    def nxt(self, name):
        r = self.rots[name]
        t = r[0][r[1] % len(r[0])]
        r[1] += 1
        return t

    def finish(self):
        self.S.emit()
        self.es.close()
        return self.nc


def _norm_rope(P_, pin, P, n, ones_l, inv_dim, gain, out, rope, epst, pS, pR):
    S = P_.S
    sq = P_.nxt("sq")
    r = P_.nxt("r")
    S.act(sq[0:P, 0:n], pin, AF.Square)
    S.mm(pS[0:P, 0:n], ones_l, sq[0:P, 0:n])
    S.act(r[0:P, 0:n], pS[0:P, 0:n], AF.Sqrt, bias=epst[0:P, :], scale=inv_dim)
    S.rcp(r[0:P, 0:n], r[0:P, 0:n])
    if rope is None:
        S.stt("v", out, pin, gain, r[0:P, 0:n], ALU.mult, ALU.mult)
        return
    R, cs, sn = rope
    kn = P_.nxt("kn")
    t1 = P_.nxt("t1")
    t2 = P_.nxt("t2")
    S.stt("v", kn[0:P, 0:n], pin, gain, r[0:P, 0:n], ALU.mult, ALU.mult)
    S.mm(pR[0:P, 0:n], R, kn[0:P, 0:n])
    S.tt("g", t1[0:P, 0:n], kn[0:P, 0:n], cs, ALU.mult)
    S.tt("v", t2[0:P, 0:n], pR[0:P, 0:n], sn, ALU.mult)
    S.tt("g", out, t1[0:P, 0:n], t2[0:P, 0:n], ALU.add)


def build_P0():
    P_ = Prog()
    S = P_.S
    cT = P_.din("cT", [128, 8, 2])
    wmod = P_.din("wmod", [1024, 6144])
    bfm = P_.din("bfm", [128, 48])
    modfm_o = P_.dout("modfm", [128, 48, 2])
    ct = P_.sb("ct", [128, 8, 2])
    sc = P_.sb("sc", [128, 8, 2])
    bt = P_.sb("bt", [128, 48])
    mf = P_.sb("mf", [128, 48, 2])
    P_.rot("wm", 2, [128, 8, 1024])
    P_.rotp("pp", 2)
    S.dma("sp", ct[:], cT)
    S.dma("sp", bt[:], bfm)
    S.act(sc[:], ct[:], AF.Silu)
    wv = wmod.rearrange("(k p) c -> p k c", p=128)
    for s_ in range(6):
        w = P_.nxt("wm")
        S.dma("sp", w[:], wv[:, :, s_ * 1024:(s_ + 1) * 1024])
        for jj in range(8):
            j = s_ * 8 + jj
            p = P_.nxt("pp")
            for k in range(8):
                S.mm(p[:, 0:2], w[:, k, jj * 128:(jj + 1) * 128], sc[:, k, :], start=(k == 0), stop=(k == 7))
            S.ts("v", mf[:, j, :], p[:, 0:2], bt[:, j:j + 1], None, ALU.add)
    S.dma("sp", modfm_o, mf[:])
    return P_.finish()


def _gs(P_, mf, nfm, base):
    S = P_.S
    G = P_.sb("Gmod", [128, 8, 2])
    S.ts("v", G[:], mf[:, (base + 1) * 8:(base + 2) * 8, :], 1.0, None, ALU.add)
    S.tt("v", G[:], G[:], nfm[:].unsqueeze(2).broadcast_to([128, 8, 2]), ALU.mult)
    return G, mf[:, base * 8:(base + 1) * 8, :]


def build_PA():
    P_ = Prog()
    S = P_.S
    x_in = P_.din("x_in", [T, 1024])
    modfm = P_.din("modfm", [128, 48, 2])
    n1 = P_.din("norm_fm", [128, 8])
    w_in = P_.din("w_in", [1024, 1952])
    cosG = P_.din("cosG", [128, T], BF16)
    sinG = P_.din("sinG", [128, T], BF16)
    cosM = P_.din("cosM", [96, T], BF16)
    sinM = P_.din("sinM", [96, T], BF16)
    gains = P_.din("gainsA", [128, 6])
    wuk = P_.din("w_uk", [128, 256])
    wuv = P_.din("w_uv", [128, 256])
    ident = P_.din("ident_bf", [128, 128], BF16)
    ones_d = P_.din("ones_bf", [128, 128], BF16)
    oblk_d = P_.din("onesblk_bf", [128, 128], BF16)
    Rg_d = P_.din("Rg", [128, 128], BF16)
    Rm_d = P_.din("Rm", [96, 96], BF16)
    gk_o = P_.dout("gk", [128, T], BF16)
    mk_o = P_.dout("mk", [4, 96, T], BF16)
    gv_o = P_.dout("gv", [128, NT, 130], BF16)
    mv_o = P_.dout("mv", [128, NT, 260], BF16)
    gq_o = P_.dout("gq", [2, 128, T], BF16)
    cq_o = P_.dout("cq", [128, 2, T], BF16)
    bg_o = P_.dout("bg", [128, 2, T], BF16)
    cu_o = P_.dout("cu", [128, 2, T], BF16)
    zp_o = P_.dout("zp", [128, 2, T], BF16)

    idt = P_.sb("idt", [128, 128], BF16)
    ones = P_.sb("ones", [128, 128], BF16)
    oblk = P_.sb("oblk", [128, 128], BF16)
    Rg = P_.sb("Rgt", [128, 128], BF16)
    Rm = P_.sb("Rmt", [96, 96], BF16)
    gn = P_.sb("gn", [128, 6])
    mf = P_.sb("mf", [128, 48, 2])
    nfm = P_.sb("nfm", [128, 8])
    epst = P_.sb("epst", [128, 1])
    wsb = P_.sb("wsb", [128, 8, 1952], BF16)
    wukp = P_.sb("wukp", [128, 4, 96], BF16)
    wkrp = P_.sb("wkrp", [128, 8, 96], BF16)
    wuvt = P_.sb("wuvt", [128, 256], BF16)
    junk = P_.sb("junk", [128, 1024], BF16)
    cgt = P_.sb("cgt", [128, 512])
    ckv = P_.sb("ckv", [128, 512], BF16)
    P_.rot("xt", 2, [128, 1024])
    P_.rot("xn", 2, [128, 1024], BF16)
    P_.rot("ss", 2, [128, 1])
    P_.rot("rstd", 2, [128, 1])
    P_.rot("hT", 2, [128, 8, 512], BF16)
    P_.rot("cg", 2, [128, 512], BF16)
    P_.rot("sg", 2, [128, 512], BF16)
    P_.rot("cm", 2, [96, 512], BF16)
    P_.rot("sm", 2, [96, 512], BF16)
    P_.rot("o2", 8, [128, 2, 512], BF16)
    P_.rot("ogk", 2, [128, 512], BF16)
    P_.rot("omk", 2, [96, 4, 512], BF16)
    ogv = [P_.sb("ogv%d" % i, [128, 4, 130], BF16) for i in range(2)]
    omv = [P_.sb("omv%d" % i, [128, 4, 260], BF16) for i in range(2)]
    P_.rot("sq", 3, [128, 512], BF16)
    P_.rot("r", 2, [128, 512])
    P_.rot("kn", 2, [128, 512], BF16)
    P_.rot("t1", 2, [128, 512])
    P_.rot("t2", 2, [128, 512])
    pT = [P_.ps("pT0", [128, 1024], BF16), P_.ps("pT1", [128, 1024], BF16)]
    P_.rotp("pp", 3)
    pS = P_.ps("pS")
    pR = P_.ps("pR")
    pV = P_.ps("pV")

    for dst, src in ((idt, ident), (ones, ones_d), (oblk, oblk_d), (Rg, Rg_d), (Rm, Rm_d), (gn, gains),
                     (mf, modfm), (nfm, n1)):
        S.dma("sp", dst[:], src)
    S.ms("v", epst[:], EPS)
    S.dma("pool", wsb[:], w_in.rearrange("(k p) c -> p k c", p=128))
    S.ms("g", wukp[:], 0.0)
    S.ms("g", wkrp[:], 0.0)
    S.dma("pool", wukp[:, :, 0:64], wuk.rearrange("p (h d) -> p h d", d=64))
    S.cp("g", wkrp[:, :, 64:96], wsb[:, :, 1920:1952])
    S.dma("pool", wuvt[:], wuv)
    for t_ in ogv + omv:
        S.ms("g", t_[:], 1.0)
    G1, S1 = _gs(P_, mf, nfm, 0)

    def proj(lo, M, hT, n, p):
        for k in range(8):
            S.mm(p[0:M, 0:n], wsb[:, k, lo:lo + M], hT[:, k, 0:n], start=(k == 0), stop=(k == 7))

    def nr(pin, P, n, ones_l, inv_dim, gain, out, rope):
        _norm_rope(P_, pin, P, n, ones_l, inv_dim, gain, out, rope, epst, pS, pR)

    for bi, (c0, n) in enumerate(BLKS):
        v = 0 if c0 < TL else 1
        hT = P_.nxt("hT")
        ntile = n // 128
        t0 = c0 // 128
        for tl in range(ntile):
            tt = t0 + tl
            xt = P_.nxt("xt")
            xn = P_.nxt("xn")
            ss = P_.nxt("ss")
            rstd = P_.nxt("rstd")
            ptb = pT[tt % 2]
            S.dma("sp", xt[:], x_in[tt * 128:(tt + 1) * 128, :])
            S.ms("v", ss[:], 0.0)
            S.act(junk[:], xt[:], AF.Square, accum=ss[:])
            S.act(rstd[:], ss[:], AF.Sqrt, bias=epst[:], scale=1.0 / 1024)
            S.rcp(rstd[:], rstd[:])
            S.ts("v", xn[:], xt[:], rstd[:], None, ALU.mult)
            for k in range(8):
                S.tr(ptb[:, k * 128:(k + 1) * 128], xn[:, k * 128:(k + 1) * 128], idt[:])
            for k in range(8):
                dst = hT[:, k, tl * 128:(tl + 1) * 128]
                src = ptb[:, k * 128:(k + 1) * 128]
                if tt % 2 == 0:
                    S.act(dst, src, AF.Identity, bias=S1[:, k, v:v + 1], scale=G1[:, k, v:v + 1])
                else:
                    S.ts("v", dst, src, G1[:, k, v:v + 1], S1[:, k, v:v + 1], ALU.mult, ALU.add)
        cg = P_.nxt("cg"); sg = P_.nxt("sg"); cm = P_.nxt("cm"); sm = P_.nxt("sm")
        S.dma("sp", cg[:, 0:n], cosG[:, c0:c0 + n])
        S.dma("sp", sg[:, 0:n], sinG[:, c0:c0 + n])
        S.dma("sp", cm[:, 0:n], cosM[:, c0:c0 + n])
        S.dma("sp", sm[:, 0:n], sinM[:, c0:c0 + n])
        ropeG = (Rg[:], cg[:, 0:n], sg[:, 0:n])
        ropeM = (Rm[:], cm[:, 0:n], sm[:, 0:n])
        o = P_.nxt("o2")
        for c in range(2):
            p = P_.nxt("pp")
            proj(c * 128, 128, hT, n, p)
            S.cp("s", o[:, c, 0:n], p[:, 0:n])
        S.dma("sp", bg_o[:, :, c0:c0 + n], o[:, :, 0:n])
        o = P_.nxt("o2")
        for c in range(2):
            p1 = P_.nxt("pp")
            proj(256 + c * 128, 128, hT, n, p1)
            S.cp("s", cgt[:, 0:n], p1[:, 0:n])
            p2 = P_.nxt("pp")
            proj(512 + c * 128, 128, hT, n, p2)
            S.tt("v", o[:, c, 0:n], p2[:, 0:n], cgt[:, 0:n], ALU.mult)
        S.dma("sp", cu_o[:, :, c0:c0 + n], o[:, :, 0:n])
        o = P_.nxt("o2")
        for c in range(2):
            p = P_.nxt("pp")
            proj(768 + c * 128, 128, hT, n, p)
            S.cp("s", o[:, c, 0:n], p[:, 0:n])
        S.dma("sp", zp_o[:, :, c0:c0 + n], o[:, :, 0:n])
        o = P_.nxt("o2")
        for c in range(2):
            p = P_.nxt("pp")
            proj(1024 + c * 128, 128, hT, n, p)
            nr(p[:, 0:n], 128, n, oblk[:], 1.0 / 64, gn[:, 0:1], o[:, c, 0:n], ropeG)
        S.dma("sp", gq_o.rearrange("c p t -> p c t")[:, :, c0:c0 + n], o[:, :, 0:n])
        p = P_.nxt("pp")
        proj(1280, 128, hT, n, p)
        ok = P_.nxt("ogk")
        nr(p[:, 0:n], 128, n, oblk[:], 1.0 / 64, gn[:, 1:2], ok[:, 0:n], ropeG)
        S.dma("sp", gk_o[:, c0:c0 + n], ok[:, 0:n])
        ov = ogv[bi % 2]
        for tl in range(ntile):
            for k in range(8):
                S.mm(pV[:, 0:128], hT[:, k, tl * 128:(tl + 1) * 128], wsb[:, k, 1408:1536], start=(k == 0), stop=(k == 7))
            S.cp("v", ov[:, tl, :].rearrange("p (h e) -> p h e", e=65)[:, :, 0:64],
                 pV[:, 0:128].rearrange("p (h d) -> p h d", d=64))
        S.dma("sp", gv_o[:, t0:t0 + ntile, :], ov[:, 0:ntile, :])
        pq = [P_.nxt("pp"), P_.nxt("pp")]
        sqs = []
        for c in range(2):
            proj(1536 + c * 128, 128, hT, n, pq[c])
            sq = P_.nxt("sq")
            S.act(sq[:, 0:n], pq[c][:, 0:n], AF.Square)
            sqs.append(sq)
        for c in range(2):
            S.mm(pS[:, 0:n], ones[:], sqs[c][:, 0:n], start=(c == 0), stop=(c == 1))
        r = P_.nxt("r")
        S.act(r[:, 0:n], pS[:, 0:n], AF.Sqrt, bias=epst[:], scale=1.0 / 256)
        S.rcp(r[:, 0:n], r[:, 0:n])
        o = P_.nxt("o2")
        for c in range(2):
            S.stt("v", o[:, c, 0:n], pq[c][:, 0:n], gn[:, 2 + c:3 + c], r[:, 0:n], ALU.mult, ALU.mult)
        S.dma("sp", cq_o[:, :, c0:c0 + n], o[:, :, 0:n])
        p = P_.nxt("pp")
        proj(1792, 128, hT, n, p)
        nr(p[:, 0:n], 128, n, ones[:], 1.0 / 128, gn[:, 4:5], ckv[:, 0:n], None)
        om = P_.nxt("omk")
        for h in range(4):
            p = P_.nxt("pp")
            S.mm(p[0:96, 0:n], wukp[:, h, :], ckv[:, 0:n], start=True, stop=False)
            for k in range(8):
                S.mm(p[0:96, 0:n], wkrp[:, k, :], hT[:, k, 0:n], start=False, stop=(k == 7))
            nr(p[0:96, 0:n], 96, n, ones[0:96, 0:96], 1.0 / 96, gn[0:96, 5:6], om[0:96, h, 0:n], ropeM)
        S.dma("sp", mk_o.rearrange("h p t -> p h t")[:, :, c0:c0 + n], om[0:96, :, 0:n])
        ov = omv[bi % 2]
        for tl in range(ntile):
            S.mm(pV[:, 0:256], ckv[:, tl * 128:(tl + 1) * 128], wuvt[:])
            S.cp("v", ov[:, tl, :].rearrange("p (h e) -> p h e", e=65)[:, :, 0:64],
                 pV[:, 0:256].rearrange("p (h d) -> p h d", d=64))
        S.dma("sp", mv_o[:, t0:t0 + ntile, :], ov[:, 0:ntile, :])
    return P_.finish()


def build_PB():
    P_ = Prog()
    S = P_.S
    x_in = P_.din("x_in", [T, 1024])
    g_rows = P_.din("g_rows", [2, 1024])
    kv_all = P_.din("kv_all", [512, KVC], BF16)
    gq_i = P_.din("gq", [2, 128, T], BF16)
    cq_i = P_.din("cq", [128, 2, T], BF16)
    bg_i = P_.din("bg", [128, 2, T], BF16)
    cu_i = P_.din("cu", [128, 2, T], BF16)
    zp_i = P_.din("zp", [128, 2, T], BF16)
    halo = P_.din("halo", [128, 2, 18], BF16)
    convw = P_.din("convw", [128, 2, 3])
    wpool = P_.din("wpool", [4, 64, 64])
    pscale = P_.din("pscale", [128, 2])
    einv_d = P_.din("edge_inv", [128, 2, 4, 8])
    wuq_d = P_.din("w_uq", [256, 384])
    mqg_d = P_.din("mq_g", [96, 1])
    cosM = P_.din("cosM", [96, T], BF16)
    sinM = P_.din("sinM", [96, T], BF16)
    w_out = P_.din("w_out", [1024, 1024])
    ones_d = P_.din("ones_bf", [128, 128], BF16)
    Rm_d = P_.din("Rm", [96, 96], BF16)
    esel_d = P_.din("esel", [65, 64])
    x_o = P_.dout("x_out", [T, 1024])

    xs = P_.sb("xs", [128, NT, 1024])
    gb = [P_.sb("g1b", [128, 1024]), P_.sb("cg1b", [128, 1024])]
    ones = P_.sb("ones", [128, 128], BF16)
    Rm = P_.sb("Rmt", [96, 96], BF16)
    esel = P_.sb("esel", [65, 64])
    epst = P_.sb("epst", [128, 1])
    P_.rot("tmp", 2, [128, 512])
    P_.rotp("pp", 2)
    P_.rotp("pS", 3)
    P_.rotp("pO", 2)
    pD = P_.ps("pD")
    S.dma("sp", xs[:], x_in.rearrange("(t p) d -> p t d", p=128))
    S.dma("sp", gb[0][:], g_rows[0:1, :].broadcast_to([128, 1024]))
    S.dma("sp", gb[1][:], g_rows[1:2, :].broadcast_to([128, 1024]))
    S.dma("sp", ones[:], ones_d)
    S.dma("sp", Rm[:], Rm_d)
    S.dma("sp", esel[:], esel_d)
    S.ms("v", epst[:], EPS)

    def outproj(nchunk, lhs, W):
        for tt in range(NT):
            g = gb[0] if tt < 16 else gb[1]
            for hf in range(2):
                p = P_.nxt("pp")
                for i in range(nchunk):
                    S.mm(p[:, :], lhs(i, tt), W(i)[:, hf * 512:(hf + 1) * 512], start=(i == 0), stop=(i == nchunk - 1))
                tmp = P_.nxt("tmp")
                S.tt("v", tmp[:], p[:, :], g[:, hf * 512:(hf + 1) * 512], ALU.mult)
                S.tt("g", xs[:, tt, hf * 512:(hf + 1) * 512], xs[:, tt, hf * 512:(hf + 1) * 512], tmp[:], ALU.add)

    with ExitStack() as es:
        cup = P_.sb("cup", [128, 2, 2308], BF16, es)
        zpp = P_.sb("zpp", [128, 2, 2336], BF16, es)
        bgt = P_.sb("bgt", [128, 2, T], BF16, es)
        mixcp = P_.sb("mixcp", [128, 4, T], BF16, es)
        dT = P_.sb("dT", [128, 2, T], BF16, es)
        tA = P_.sb("tA", [128, 2080], F32, es)
        tB = P_.sb("tB", [128, 2080], F32, es)
        tE = P_.sb("tE", [128, 8], F32, es)
        hl = P_.sb("hl", [128, 2, 18], BF16, es)
        cw = P_.sb("cw", [128, 2, 3], F32, es)
        psc = P_.sb("psc", [128, 2], F32, es)
        einv = P_.sb("einv", [128, 2, 4, 8], F32, es)
        wpbd = P_.sb("wpbd", [128, 2, 128], BF16, es)
        wocp = P_.sb("wocp", [128, 4, 1024], BF16, es)
        S.ms("g", cup[:], 0.0)
        S.ms("g", zpp[:], 0.0)
        S.ms("g", wpbd[:], 0.0)
        S.dma("sp", hl[:], halo)
        S.dma("sp", cw[:], convw)
        S.dma("sp", psc[:], pscale)
        S.dma("sp", einv[:], einv_d)
        S.dma("sp", bgt[:], bg_i)
        S.dma("sp", cup[:, :, 1:2049], cu_i[:, :, 0:TL])
        S.dma("sp", cup[:, :, 2051:2307], cu_i[:, :, TL:T])
        S.dma("sp", zpp[:, :, 8:2056], zp_i[:, :, 0:TL])
        S.dma("sp", zpp[:, :, 2072:2328], zp_i[:, :, TL:T])
        S.cp("g", cup[:, :, 0:1], hl[:, :, 0:1])
        S.cp("g", cup[:, :, 2049:2050], hl[:, :, 1:2])
        S.cp("g", zpp[:, :, 0:8], hl[:, :, 2:10])
        S.cp("g", zpp[:, :, 2056:2064], hl[:, :, 10:18])
        for g in range(4):
            r0 = (g % 2) * 64
            S.dma("pool", wpbd[r0:r0 + 64, g // 2, r0:r0 + 64], wpool[g])
        S.dma("pool", wocp[:], w_out[0:512, :].rearrange("(k p) c -> p k c", p=128))
        segs = [(0, 0, TL, 0), (2050, 2064, 256, TL)]
        for c in range(2):
            for (co, zo, L, m0) in segs:
                S.ts("v", tA[:, 0:L], cup[:, c, co + 1:co + 1 + L], cw[:, c, 1:2], None, ALU.mult)
                S.stt("v", tA[:, 0:L], cup[:, c, co:co + L], cw[:, c, 0:1], tA[:, 0:L], ALU.mult, ALU.add)
                S.stt("v", tA[:, 0:L], cup[:, c, co + 2:co + 2 + L], cw[:, c, 2:3], tA[:, 0:L], ALU.mult, ALU.add)
                S.tt("g", mixcp[:, c, m0:m0 + L], tA[:, 0:L], bgt[:, c, m0:m0 + L], ALU.mult)
        for c in range(2):
            for hh in range(2):
                g = 2 * c + hh
                w = 2 ** (g + 1)
                rs = slice(hh * 64, hh * 64 + 64)
                for si, (co, zo, L, m0) in enumerate(segs):
                    Z = zpp[rs, c, zo:zo + L + 16]
                    A = tA[rs, 0:L + 16]
                    B = tB[rs, 0:L + 16]
                    S.tt("v", A[:, 1:L + 16], Z[:, 0:L + 15], Z[:, 1:L + 16], ALU.add)
                    Sw = A
                    if w >= 4:
                        S.tt("v", B[:, 2:L + 15], A[:, 1:L + 14], A[:, 3:L + 16], ALU.add)
                        Sw = B
                    if w >= 8:
                        S.tt("v", A[:, 4:L + 13], B[:, 2:L + 11], B[:, 6:L + 15], ALU.add)
                        Sw = A
                    if w >= 16:
                        S.tt("v", B[:, 8:L + 8], A[:, 4:L + 4], A[:, 12:L + 12], ALU.add)
                        Sw = B
                    S.stt("v", dT[rs, c, m0:m0 + L], Sw[:, 8:8 + L], 1.0 / w, Z[:, 8:8 + L], ALU.mult, ALU.subtract)
                    for ei, a in ((2 * si, 0), (2 * si + 1, L - 8)):
                        S.tt("v", tE[rs, :], Sw[:, 8 + a:16 + a], einv[rs, c, ei, :], ALU.mult)
                        S.tt("v", dT[rs, c, m0 + a:m0 + a + 8], tE[rs, :], Z[:, 8 + a:16 + a], ALU.subtract)
        for c in range(2):
            for (c0, n) in BLKS:
                p = P_.nxt("pp")
                S.mm(p[:, 0:n], wpbd[:, c, :], dT[:, c, c0:c0 + n])
                S.ts("v", mixcp[:, 2 + c, c0:c0 + n], p[:, 0:n], psc[:, c:c + 1], None, ALU.mult)
        outproj(4, lambda i, tt: mixcp[:, i, tt * 128:(tt + 1) * 128], lambda i: wocp[:, i, :])

    def attn(heads, scale, mixh):
        for (c0, n) in BLKS:
            kbs = list(range(66)) if c0 < TL else [0, 1]
            pos = [P_.nxt("pO") for _ in heads]
            for kb in kbs:
                pts = []
                for (Q, K, V, slot) in heads:
                    pSb = P_.nxt("pS")
                    S.mm(pSb[:, 0:n], K(kb), Q(c0, n))
                    pt = P_.nxt("pt")
                    S.act(pt[:, 0:n], pSb[:, 0:n], AF.Exp, scale=scale)
                    pts.append(pt)
                for (Q, K, V, slot), pt, po in zip(heads, pts, pos):
                    S.mm(po[0:65, 0:n], V(kb), pt[:, 0:n], start=(kb == kbs[0]), stop=(kb == kbs[-1]))
            for (Q, K, V, slot), po in zip(heads, pos):
                osb = P_.nxt("osb")
                S.cp("v", osb[0:65, 0:n], po[0:65, 0:n])
                S.mm(pD[0:64, 0:n], esel[0:65, :], osb[0:65, 0:n])
                rd = P_.nxt("rd")
                S.rcp(rd[0:64, 0:n], pD[0:64, 0:n])
                S.tt("g", mixh[0:64, slot, c0:c0 + n], osb[0:64, 0:n], rd[0:64, 0:n], ALU.mult)

    with ExitStack() as es:
        P_.rot("pt", 4, [128, 512], BF16, es)
        P_.rot("osb", 2, [65, 512], F32, es)
        P_.rot("rd", 2, [64, 512], F32, es)
        mixh = P_.sb("mixh", [64, 4, T], BF16, es)
        woh = P_.sb("woh", [64, 4, 1024], BF16, es)
        with ExitStack() as es2:
            gK = P_.sb("gK", [128, 66 * 128], BF16, es2)
            gV = P_.sb("gV", [128, 66, 130], BF16, es2)
            gqt = P_.sb("gqt", [128, 2, T], BF16, es2)
            S.dma("sp", gqt[:], gq_i.rearrange("c p t -> p c t"))
            S.dma("sp", gK[:, 0:256], kv_all[0:128, TL:T])
            S.dma("sp", gV[:, 0:2, :], kv_all[0:128, GV0 + 16 * 130:GV0 + 18 * 130].rearrange("p (t e) -> p t e", e=130))
            for r in range(4):
                S.dma("sp", gK[:, 256 + r * TL:256 + (r + 1) * TL], kv_all[r * 128:(r + 1) * 128, 0:TL])
                S.dma("sp", gV[:, 2 + 16 * r:18 + 16 * r, :],
                      kv_all[r * 128:(r + 1) * 128, GV0:GV0 + 16 * 130].rearrange("p (t e) -> p t e", e=130))
            S.dma("pool", woh[:], w_out[512:768, :].rearrange("(h d) c -> d h c", d=64))
            for c in range(2):
                heads = []
                for r0 in (0, 64):
                    kvh = r0 // 64
                    heads.append((
                        (lambda c0, n, c=c, r0=r0: gqt[r0:r0 + 64, c, c0:c0 + n]),
                        (lambda kb, r0=r0: gK[r0:r0 + 64, kb * 128:(kb + 1) * 128]),
                        (lambda kb, kvh=kvh: gV[:, kb, kvh * 65:(kvh + 1) * 65]),
                        (0 if r0 == 0 else 2) + c))
                attn(heads, GQA_SCALE, mixh)
        outproj(4, lambda i, tt: mixh[0:64, i, tt * 128:(tt + 1) * 128], lambda i: woh[0:64, i, :])
        with ExitStack() as es2:
            cqt = P_.sb("cqt", [128, 2, T], BF16, es2)
            wuq = P_.sb("wuq", [128, 2, 384], BF16, es2)
            mqg = P_.sb("mqg", [96, 1], F32, es2)
            P_.rot("cmt", 2, [96, 512], BF16, es2)
            P_.rot("smt", 2, [96, 512], BF16, es2)
            P_.rot("mK", 2, [96, 66 * 128], BF16, es2)
            P_.rot("mV", 2, [128, 66, 65], BF16, es2)
            P_.rot("mq", 1, [96, T], BF16, es2)
            P_.rot("sq", 1, [128, 512], BF16, es2)
            P_.rot("r", 1, [128, 512], F32, es2)
            P_.rot("kn", 1, [128, 512], BF16, es2)
            P_.rot("t1", 1, [128, 512], F32, es2)
            P_.rot("t2", 1, [128, 512], F32, es2)
            S.dma("sp", cqt[:], cq_i)
            S.dma("pool", wuq[:], wuq_d.rearrange("(k p) c -> p k c", p=128))
            S.dma("sp", mqg[:], mqg_d)
            S.dma("pool", woh[:], w_out[768:1024, :].rearrange("(h d) c -> d h c", d=64))
            for h in range(4):
                mK = P_.nxt("mK")
                mV = P_.nxt("mV")
                mq = P_.nxt("mq")
                kc = T + h * T
                S.dma("sp", mK[:, 0:256], kv_all[0:96, kc + TL:kc + T])
                S.dma("sp", mV[:, 0:2, :],
                      kv_all[0:128, MV0 + 16 * 260:MV0 + 18 * 260].rearrange("p (t e) -> p t e", e=260)[:, :, h * 65:(h + 1) * 65])
                for r in range(4):
                    S.dma("sp", mK[:, 256 + r * TL:256 + (r + 1) * TL], kv_all[r * 128:r * 128 + 96, kc:kc + TL])
                    S.dma("sp", mV[:, 2 + 16 * r:18 + 16 * r, :],
                          kv_all[r * 128:(r + 1) * 128, MV0:MV0 + 16 * 260].rearrange("p (t e) -> p t e", e=260)[:, :, h * 65:(h + 1) * 65])
                for (c0, n) in BLKS:
                    p = P_.nxt("pp")
                    for c in range(2):
                        S.mm(p[0:96, 0:n], wuq[:, c, h * 96:(h + 1) * 96], cqt[:, c, c0:c0 + n], start=(c == 0), stop=(c == 1))
                    cmt = P_.nxt("cmt")
                    smt = P_.nxt("smt")
                    S.dma("sp", cmt[:, 0:n], cosM[:, c0:c0 + n])
                    S.dma("sp", smt[:, 0:n], sinM[:, c0:c0 + n])
                    _norm_rope(P_, p[0:96, 0:n], 96, n, ones[0:96, 0:96], 1.0 / 96, mqg[:, 0:1], mq[0:96, c0:c0 + n],
                               (Rm[:], cmt[:, 0:n], smt[:, 0:n]), epst, pD, P_.nxt("pp"))
                attn([((lambda c0, n, mq=mq: mq[0:96, c0:c0 + n]),
                       (lambda kb, mK=mK: mK[0:96, kb * 128:(kb + 1) * 128]),
                       (lambda kb, mV=mV: mV[:, kb, 0:65]), h)], MLA_SCALE, mixh)
        outproj(4, lambda i, tt: mixh[0:64, i, tt * 128:(tt + 1) * 128], lambda i: woh[0:64, i, :])
    S.dma("sp", x_o.rearrange("(t p) d -> p t d", p=128), xs[:])
    return P_.finish()


def build_PC():
    P_ = Prog()
    S = P_.S
    HT = 9
    HC = HT * 128
    x_in = P_.din("x_in", [T, 1024])
    modfm = P_.din("modfm", [128, 48, 2])
    n2 = P_.din("norm_fm", [128, 8])
    g_rows = P_.din("g_rows", [2, 1024])
    wr_d = P_.din("w_router", [1024, 16])
    br_d = P_.din("b_router", [1, 16])
    wg_d = P_.din("w_gate", [16, 1024, 1024])
    wu_d = P_.din("w_up", [16, 1024, 1024])
    wd_d = P_.din("w_down", [16, 1024, 1024])
    identf = P_.din("ident_f32", [128, 128])
    x_o = P_.dout("x_out", [T, 1024])

    idf = P_.sb("idf", [128, 128])
    mf = P_.sb("mf", [128, 48, 2])
    nfm = P_.sb("nfm", [128, 8])
    epst = P_.sb("epst", [128, 1])
    gb = [P_.sb("g2b", [128, 1024]), P_.sb("cg2b", [128, 1024])]
    wr = P_.sb("wr", [128, 8, 16])
    brt = P_.sb("brt", [128, 16])
    xs = P_.sb("xs", [128, HT, 1024])
    h2T = P_.sb("h2T", [128, 8, HC], BF16)
    h2f = P_.sb("h2f", [128, 8, 128])
    junk = P_.sb("junk", [128, 1024], BF16)
    xn = P_.sb("xn", [128, 1024])
    lg = P_.sb("lg", [128, HT, 16])
    P_.rot("ss", 2, [128, 1])
    P_.rot("rstd", 2, [128, 1])
    P_.rot("wg", 2, [128, 8, 1024], BF16)
    P_.rot("wu", 2, [128, 8, 1024], BF16)
    P_.rot("wd", 2, [128, 8, 1024], BF16)
    P_.rot("actT", 2, [128, 8, 512], BF16)
    P_.rot("sgt", 2, [128, 512], BF16)
    P_.rot("tmp", 2, [128, 512])
    pT = P_.ps("pT", [128, 1024], F32)
    pL = P_.ps("pL")
    P_.rotp("pg", 2)
    P_.rotp("pd", 2)
    rt = {nm: P_.sb("rt_" + nm, shp) for nm, shp in (
        ("sc", [128, HT, 16]), ("sel", [128, HT, 16]), ("m1", [128, HT, 4]), ("eq", [128, HT, 16]),
        ("v2", [128, HT, 16]), ("m2", [128, HT, 4]), ("gs", [128, HT, 4]), ("gmax", [128, HT]),
        ("gmask", [128, HT, 4]), ("t2m", [128, HT, 16]), ("sm", [128, HT, 16]), ("ssum", [128, HT]),
        ("gw", [128, HT, 16]))}

    S.dma("sp", idf[:], identf)
    S.dma("sp", mf[:], modfm)
    S.dma("sp", nfm[:], n2)
    S.dma("sp", gb[0][:], g_rows[0:1, :].broadcast_to([128, 1024]))
    S.dma("sp", gb[1][:], g_rows[1:2, :].broadcast_to([128, 1024]))
    S.dma("sp", wr[:], wr_d.rearrange("(k p) e -> p k e", p=128))
    S.dma("sp", brt[:], br_d.broadcast_to([128, 16]))
    S.ms("v", epst[:], EPS)
    G2, S2 = _gs(P_, mf, nfm, 3)
    wgv = wg_d.rearrange("e (k p) c -> e p k c", p=128)
    wuv = wu_d.rearrange("e (k p) c -> e p k c", p=128)
    wdv = wd_d.rearrange("e (k p) c -> e p k c", p=128)
    r4 = lambda a: a.rearrange("p t (g e) -> p t g e", e=4)

    for half in range(2):
        tb = half * HT
        S.dma("sp", xs[:], x_in[tb * 128:(tb + HT) * 128, :].rearrange("(t p) d -> p t d", p=128))
        for tl in range(HT):
            tt = tb + tl
            v = 0 if tt < 16 else 1
            ss = P_.nxt("ss")
            rstd = P_.nxt("rstd")
            S.ms("v", ss[:], 0.0)
            S.act(junk[:], xs[:, tl, :], AF.Square, accum=ss[:])
            S.act(rstd[:], ss[:], AF.Sqrt, bias=epst[:], scale=1.0 / 1024)
            S.rcp(rstd[:], rstd[:])
            S.ts("v", xn[:], xs[:, tl, :], rstd[:], None, ALU.mult)
            for k in range(8):
                if k % 4 == 0:
                    pass
                S.tr(pT[:, (k % 8) * 128:(k % 8 + 1) * 128], xn[:, k * 128:(k + 1) * 128], idf[:])
            for k in range(8):
                S.ts("v", h2f[:, k, :], pT[:, k * 128:(k + 1) * 128], G2[:, k, v:v + 1], S2[:, k, v:v + 1], ALU.mult, ALU.add)
            S.cp("g", h2T[:, :, tl * 128:(tl + 1) * 128], h2f[:])
            for k in range(8):
                S.mm(pL[:, 0:16], h2f[:, k, :], wr[:, k, :], start=(k == 0), stop=(k == 7))
            S.cp("v", lg[:, tl, :], pL[:, 0:16])
        S.act(rt["sc"][:], lg[:], AF.Sigmoid)
        S.tt("v", rt["sel"][:], rt["sc"][:], brt[:].unsqueeze(1).broadcast_to([128, HT, 16]), ALU.add)
        S.red(rt["m1"][:], r4(rt["sel"][:]), ALU.max)
        S.tt("v", r4(rt["eq"][:]), r4(rt["sel"][:]), rt["m1"][:].unsqueeze(3).broadcast_to([128, HT, 4, 4]), ALU.is_equal)
        S.stt("v", rt["v2"][:], rt["eq"][:], -1e9, rt["sel"][:], ALU.mult, ALU.add)
        S.red(rt["m2"][:], r4(rt["v2"][:]), ALU.max)
        S.tt("v", rt["gs"][:], rt["m1"][:], rt["m2"][:], ALU.add)
        S.red(rt["gmax"][:], rt["gs"][:], ALU.max)
        S.tt("v", rt["gmask"][:], rt["gs"][:], rt["gmax"][:].unsqueeze(2).broadcast_to([128, HT, 4]), ALU.is_equal)
        S.tt("v", r4(rt["t2m"][:]), r4(rt["sel"][:]), rt["m2"][:].unsqueeze(3).broadcast_to([128, HT, 4, 4]), ALU.is_ge)
        S.tt("v", r4(rt["t2m"][:]), r4(rt["t2m"][:]), rt["gmask"][:].unsqueeze(3).broadcast_to([128, HT, 4, 4]), ALU.mult)
        S.tt("v", rt["sm"][:], rt["sc"][:], rt["t2m"][:], ALU.mult)
        S.red(rt["ssum"][:], rt["sm"][:], ALU.add)
        S.rcp(rt["ssum"][:], rt["ssum"][:])
        S.tt("v", rt["gw"][:], rt["sm"][:], rt["ssum"][:].unsqueeze(2).broadcast_to([128, HT, 16]), ALU.mult)
        gw = rt["gw"]
        cur = None
        for e in range(16):
            if cur is None:
                cur = (P_.nxt("wg"), P_.nxt("wu"), P_.nxt("wd"))
                S.dma("pool", cur[0][:], wgv[e]); S.dma("pool", cur[1][:], wuv[e]); S.dma("pool", cur[2][:], wdv[e])
            wg, wu, wd = cur
            if e + 1 < 16:
                nx = (P_.nxt("wg"), P_.nxt("wu"), P_.nxt("wd"))
                S.dma("pool", nx[0][:], wgv[e + 1]); S.dma("pool", nx[1][:], wuv[e + 1]); S.dma("pool", nx[2][:], wdv[e + 1])
            else:
                nx = None
            for (c0, n) in ((0, 512), (512, 512), (1024, 128)):
                aT = P_.nxt("actT")
                for m in range(8):
                    pg = P_.nxt("pg")
                    pu = P_.nxt("pg")
                    for k in range(8):
                        S.mm(pg[:, 0:n], wg[:, k, m * 128:(m + 1) * 128], h2T[:, k, c0:c0 + n], start=(k == 0), stop=(k == 7))
                    for k in range(8):
                        S.mm(pu[:, 0:n], wu[:, k, m * 128:(m + 1) * 128], h2T[:, k, c0:c0 + n], start=(k == 0), stop=(k == 7))
                    sg = P_.nxt("sgt")
                    S.act(sg[:, 0:n], pg[:, 0:n], AF.Silu)
                    S.tt("v", aT[:, m, 0:n], pu[:, 0:n], sg[:, 0:n], ALU.mult)
                for tl2 in range(n // 128):
                    tl = c0 // 128 + tl2
                    tt = tb + tl
                    g = gb[0] if tt < 16 else gb[1]
                    for hf in range(2):
                        pd = P_.nxt("pd")
                        for m in range(8):
                            S.mm(pd[:, :], aT[:, m, tl2 * 128:(tl2 + 1) * 128], wd[:, m, hf * 512:(hf + 1) * 512], start=(m == 0), stop=(m == 7))
                        tmp = P_.nxt("tmp")
                        S.stt("v", tmp[:], pd[:, :], gw[:, tl, e:e + 1], g[:, hf * 512:(hf + 1) * 512], ALU.mult, ALU.mult)
                        S.tt("g", xs[:, tl, hf * 512:(hf + 1) * 512], xs[:, tl, hf * 512:(hf + 1) * 512], tmp[:], ALU.add)
            cur = nx
        S.dma("sp", x_o[tb * 128:(tb + HT) * 128, :].rearrange("(t p) d -> p t d", p=128), xs[:])
    return P_.finish()


_PROGS = {}


def _prog(name):
    if name not in _PROGS:
        _PROGS[name] = {"P0": build_P0, "PA": build_PA, "PB": build_PB, "PC": build_PC}[name]()
    return _PROGS[name]


def _run(name, in_maps):
    nc = _prog(name)
    res = run_bass_kernel_spmd(nc, in_maps, core_ids=list(range(8)))
    return res.results


def _fm(vec, p=128):
    v = np.asarray(vec, np.float32)
    return np.ascontiguousarray(v.reshape(-1, p).T)


def _tables(t0):
    pos = t0 + np.arange(TL)
    row = (pos // 64).astype(np.float32)
    col = (pos % 64).astype(np.float32)

    def tab(P, rot0, half, nfreq):
        cs = np.ones((P, T), np.float32)
        sn = np.zeros((P, T), np.float32)
        inv = np.power(np.float32(10000.0), -np.arange(nfreq, dtype=np.float32) / np.float32(nfreq)).astype(np.float32)
        for p in range(P):
            i = p % 64 if P == 128 else p
            if i < rot0:
                continue
            j = (i - rot0) % half
            f = j % nfreq
            ang = (row if j < nfreq else col) * inv[f]
            cs[p, :TL] = np.cos(ang)
            sn[p, :TL] = np.sin(ang)
        return cs.astype(NPBF), sn.astype(NPBF)

    cg, sg = tab(128, 0, 32, 16)
    cm, sm = tab(96, 64, 16, 8)
    return cg, sg, cm, sm


def _consts():
    ident = np.eye(128, dtype=np.float32)
    oblk = np.zeros((128, 128), np.float32)
    oblk[:64, :64] = 1
    oblk[64:, 64:] = 1
    Rg = np.zeros((128, 128), np.float32)
    for b in (0, 64):
        for m in range(32):
            Rg[b + m + 32, b + m] = -1
            Rg[b + m, b + m + 32] = 1
    Rm = np.zeros((96, 96), np.float32)
    for m in range(16):
        Rm[64 + m + 16, 64 + m] = -1
        Rm[64 + m, 64 + m + 16] = 1
    esel = np.zeros((65, 64), np.float32)
    esel[64, :] = 1
    return dict(ident_bf=ident.astype(NPBF), ident_f32=ident, ones_bf=np.ones((128, 128), NPBF),
                onesblk_bf=oblk.astype(NPBF), Rg=Rg.astype(NPBF), Rm=Rm.astype(NPBF), esel=esel)


def _edge_inv(t0):
    out = np.zeros((128, 2, 4, 8), np.float32)
    for c in range(2):
        for p in range(128):
            w = 2 ** (2 * c + p // 64 + 1)
            for ei, (base, n) in enumerate(((t0, 8192), (t0 + TL - 8, 8192), (0, 256), (248, 256))):
                t = base + np.arange(8)
                cnt = np.minimum(t + w // 2, n) - np.maximum(t - w // 2, 0)
                out[p, c, ei] = 1.0 / cnt.astype(np.float32)
    return out


def kernel(x, c, ctx, c_ctx, w_mod, b_mod, norm1, norm2, w_in, conv_w, w_pool, pool_scale,
           gqa_q_norm, gqa_k_norm, mla_q_norm, mla_kv_norm, mla_w_uq, mla_w_uk, mla_w_uv,
           mla_qk_q_norm, mla_qk_k_norm, w_out, w_router, b_router, w_gate, w_up, w_down):
    f32 = lambda a: np.ascontiguousarray(np.asarray(a, dtype=np.float32))
    x = f32(x); c = f32(c); ctx = f32(ctx); c_ctx = f32(c_ctx)
    w_mod = f32(w_mod); b_mod = f32(b_mod); w_in = f32(w_in); w_out = f32(w_out)
    w_gate = f32(w_gate); w_up = f32(w_up); w_down = f32(w_down)
    cst = _consts()
    cores = [(b, r) for b in range(2) for r in range(4)]
    tabs = [_tables(r * TL) for (b, r) in cores]
    einv = [_edge_inv(r * TL) for (b, r) in cores]
    xs = [np.concatenate([x[b, r * TL:(r + 1) * TL], ctx[b]], axis=0) for (b, r) in cores]
    cT = [np.ascontiguousarray(np.stack([_fm(c[b]), _fm(c_ctx)], axis=-1)) for (b, r) in cores]
    perm = np.arange(1952)
    perm[1024:1280] = 1024 + np.concatenate([np.arange(0, 64), np.arange(128, 192), np.arange(64, 128), np.arange(192, 256)])
    for l in range(2):
        r0 = _run("P0", [dict(cT=cT[i], wmod=w_mod[l], bfm=_fm(b_mod[l])) for i in range(8)])
        modfm = [np.asarray(r0[i]["modfm"], np.float32) for i in range(8)]

        def grow(i, sec):
            m = modfm[i]
            return np.ascontiguousarray(np.stack([m[:, sec * 8:(sec + 1) * 8, v].T.reshape(-1) for v in (0, 1)]))
        w_in_p = np.ascontiguousarray(w_in[l][:, perm])
        gains = np.zeros((128, 6), np.float32)
        gains[:, 0] = np.tile(np.asarray(gqa_q_norm[l], np.float32), 2)
        gains[:, 1] = np.tile(np.asarray(gqa_k_norm[l], np.float32), 2)
        gains[:, 2:4] = _fm(mla_q_norm[l])
        gains[:, 4] = np.asarray(mla_kv_norm[l], np.float32)
        gains[:96, 5] = np.asarray(mla_qk_k_norm[l], np.float32)
        ra = _run("PA", [dict(x_in=xs[i], modfm=modfm[i], norm_fm=_fm(norm1[l]), w_in=w_in_p,
                              cosG=tabs[i][0], sinG=tabs[i][1], cosM=tabs[i][2], sinM=tabs[i][3],
                              gainsA=gains, w_uk=f32(mla_w_uk[l]), w_uv=f32(mla_w_uv[l]),
                              ident_bf=cst["ident_bf"], ones_bf=cst["ones_bf"], onesblk_bf=cst["onesblk_bf"],
                              Rg=cst["Rg"], Rm=cst["Rm"]) for i in range(8)])
        pay = []
        for i in range(8):
            o = ra[i]
            p = np.zeros((128, KVC), NPBF)
            p[:, 0:T] = o["gk"]
            for h in range(4):
                p[0:96, T * (1 + h):T * (2 + h)] = o["mk"][h]
            p[:, GV0:MV0] = np.asarray(o["gv"]).reshape(128, NT * 130)
            p[:, MV0:KVC] = np.asarray(o["mv"]).reshape(128, NT * 260)
            pay.append(p)
        kv_all = [np.concatenate(pay[b * 4:(b + 1) * 4], axis=0) for b in range(2)]
        halos = []
        for i, (b, r) in enumerate(cores):
            h = np.zeros((128, 2, 18), NPBF)
            if r > 0:
                h[:, :, 0] = ra[i - 1]["cu"][:, :, TL - 1]
                h[:, :, 2:10] = ra[i - 1]["zp"][:, :, TL - 8:TL]
            if r < 3:
                h[:, :, 1] = ra[i + 1]["cu"][:, :, 0]
                h[:, :, 10:18] = ra[i + 1]["zp"][:, :, 0:8]
            halos.append(h)
        cw = np.ascontiguousarray(np.asarray(conv_w[l], np.float32).reshape(3, 2, 128).transpose(2, 1, 0))
        rb = _run("PB", [dict(x_in=xs[i], g_rows=grow(i, 2), kv_all=kv_all[cores[i][0]], gq=ra[i]["gq"], cq=ra[i]["cq"],
                              bg=ra[i]["bg"], cu=ra[i]["cu"], zp=ra[i]["zp"], halo=halos[i], convw=cw,
                              wpool=f32(w_pool[l]), pscale=_fm(pool_scale[l]), edge_inv=einv[i],
                              w_uq=f32(mla_w_uq[l]), mq_g=f32(mla_qk_q_norm[l]).reshape(96, 1),
                              cosM=tabs[i][2], sinM=tabs[i][3], w_out=w_out[l], ones_bf=cst["ones_bf"],
                              Rm=cst["Rm"], esel=cst["esel"]) for i in range(8)])
        xm = [np.asarray(rb[i]["x_out"], np.float32) for i in range(8)]
        rc = _run("PC", [dict(x_in=xm[i], modfm=modfm[i], norm_fm=_fm(norm2[l]), g_rows=grow(i, 5),
                              w_router=f32(w_router), b_router=f32(b_router).reshape(1, 16),
                              w_gate=w_gate[l], w_up=w_up[l], w_down=w_down[l], ident_f32=cst["ident_f32"])
                         for i in range(8)])
        xs = [np.asarray(rc[i]["x_out"], np.float32) for i in range(8)]
    out = np.zeros((2, 8192, 1024), np.float32)
    for i, (b, r) in enumerate(cores):
        out[b, r * TL:(r + 1) * TL] = xs[i][0:TL]
    return out
```
